# Optimizing a Trainium2 kernel written in Bass

```python
import jax, jax.numpy as jnp
from jax import lax
import numpy as np

D_MODEL = 1024
BATCH = 16
SEQ = 2048
DEPTH = 1

POOL_WIDTH = D_MODEL // 2
POOL_WINDOWS = (2, 4, 8, 16)
N_POOL_GROUPS = len(POOL_WINDOWS)
POOL_GROUP = POOL_WIDTH // N_POOL_GROUPS
N_HEADS = 8
HEAD_DIM = 64
ATTN_WIDTH = N_HEADS * HEAD_DIM
MOBA_BLOCK = 256
MOBA_TOPK = 3
Q_CHUNK = 32
D_FF = 4 * D_MODEL
N_BRANCHES = 2
IN_WIDTH = POOL_WIDTH + 3 * ATTN_WIDTH + N_BRANCHES * D_MODEL
EPS = 1e-6
NEG = -1e30

kernel_name = "hybrid_pool_moba_gated_block"


def rmsnorm(x, g):
    xf = x.astype(jnp.float32)
    y = xf * lax.rsqrt(jnp.mean(xf * xf, axis=-1, keepdims=True) + EPS) * g.astype(jnp.float32)
    return y.astype(x.dtype)


def alibi_slopes(n_heads):
    return jnp.asarray(2.0 ** (-8.0 * np.arange(1, n_heads + 1) / n_heads), dtype=jnp.float32)


def pool_mixer(p, pool_w, pool_scale):
    pf = p.astype(jnp.float32)
    B, S, _ = pf.shape
    cs = jnp.concatenate([jnp.zeros_like(pf[:, :1]), jnp.cumsum(pf, axis=1)], axis=1)
    t = jnp.arange(S)
    outs = []
    for g, w in enumerate(POOL_WINDOWS):
        sl = slice(g * POOL_GROUP, (g + 1) * POOL_GROUP)
        lo = jnp.maximum(t + 1 - w, 0)
        win_sum = cs[:, 1:, sl] - cs[:, lo, sl]
        count = jnp.minimum(t + 1, w).astype(jnp.float32)[None, :, None]
        outs.append(win_sum / count - pf[:, :, sl])
    y = jnp.stack(outs, axis=2)
    y = jnp.einsum('bsgc,gcd->bsgd', y, pool_w.astype(jnp.float32))
    y = y.reshape(B, S, POOL_WIDTH) * pool_scale.astype(jnp.float32)
    return y.astype(p.dtype)


def moba_attention(q, k, v):
    B, S, H, Dh = q.shape
    nb = -(-S // MOBA_BLOCK)
    s_pad = nb * MOBA_BLOCK
    qf = q.transpose(0, 2, 1, 3).astype(jnp.float32) * (HEAD_DIM ** -0.5)
    pad = ((0, 0), (0, 0), (0, s_pad - S), (0, 0))
    kf = jnp.pad(k.transpose(0, 2, 1, 3).astype(jnp.float32), pad)
    vf = jnp.pad(v.transpose(0, 2, 1, 3).astype(jnp.float32), pad)
    kb = kf.reshape(B, H, nb, MOBA_BLOCK, Dh)
    vb = vf.reshape(B, H, nb, MOBA_BLOCK, Dh)
    slope = alibi_slopes(H)[None, :, None, None]
    t = jnp.arange(S)
    qblk = t // MOBA_BLOCK
    k_sel = min(MOBA_TOPK, nb - 1)
    if k_sel > 0:
        kmean = jnp.mean(kb, axis=3)
        gate = jnp.einsum('bhsd,bhnd->bhsn', qf, kmean)
        past = jnp.arange(nb)[None, :] < qblk[:, None]
        gate = jnp.where(past[None, None], gate, NEG)
        _, sel = lax.top_k(gate, k_sel)
    else:
        sel = jnp.zeros((B, H, S, 0), dtype=jnp.int32)
    n_chunks = S // Q_CHUNK
    q_c = qf.reshape(B, H, n_chunks, Q_CHUNK, Dh).transpose(2, 0, 1, 3, 4)
    sel_c = sel.reshape(B, H, n_chunks, Q_CHUNK, k_sel).transpose(2, 0, 1, 3, 4)
    bi = jnp.arange(B)[:, None, None]
    hi = jnp.arange(H)[None, :, None]
    kpos_in = jnp.arange(MOBA_BLOCK)

    def chunk_attn(args):
        c, qc, selc = args
        tq = c * Q_CHUNK + jnp.arange(Q_CHUNK)
        own = (c * Q_CHUNK) // MOBA_BLOCK
        k_own = lax.dynamic_slice_in_dim(kf, own * MOBA_BLOCK, MOBA_BLOCK, axis=2)
        v_own = lax.dynamic_slice_in_dim(vf, own * MOBA_BLOCK, MOBA_BLOCK, axis=2)
        ks = own * MOBA_BLOCK + kpos_in
        dist = (tq[:, None] - ks[None, :]).astype(jnp.float32)
        s_own = jnp.einsum('bhqd,bhkd->bhqk', qc, k_own) - slope * dist[None, None]
        s_own = jnp.where((ks[None, :] <= tq[:, None])[None, None], s_own, NEG)
        scores = [s_own]
        idxs = []
        for j in range(k_sel):
            idx = selc[..., j]
            kg = kb[bi, hi, idx]
            kpos = idx[..., None] * MOBA_BLOCK + kpos_in
            d = (tq[None, None, :, None] - kpos).astype(jnp.float32)
            s = jnp.einsum('bhqd,bhqkd->bhqk', qc, kg) - slope * d
            s = jnp.where((idx < own)[..., None], s, NEG)
            scores.append(s)
            idxs.append(idx)
        p = jax.nn.softmax(jnp.concatenate(scores, axis=-1), axis=-1)
        out = jnp.einsum('bhqk,bhkd->bhqd', p[..., :MOBA_BLOCK], v_own)
        for j, idx in enumerate(idxs):
            vg = vb[bi, hi, idx]
            pj = p[..., (j + 1) * MOBA_BLOCK:(j + 2) * MOBA_BLOCK]
            out = out + jnp.einsum('bhqk,bhqkd->bhqd', pj, vg)
        return out

    out = lax.map(chunk_attn, (jnp.arange(n_chunks), q_c, sel_c))
    out = out.transpose(1, 0, 3, 2, 4).reshape(B, S, H * Dh)
    return out.astype(q.dtype)


def setup_inputs(seed: int = 0) -> dict:
    key = jax.random.key(seed)
    ks = jax.random.split(key, 16)
    f32 = jnp.float32

    def nrm(k, shape, fan_in):
        return jax.random.normal(k, shape, f32) * (fan_in ** -0.5)

    def gain(k, shape):
        return 1.0 + 0.02 * jax.random.normal(k, shape, f32)

    L = DEPTH
    return {
        "x": jax.random.normal(ks[0], (BATCH, SEQ, D_MODEL), f32),
        "norm_mix_pre": gain(ks[1], (L, D_MODEL)),
        "w_in": nrm(ks[2], (L, D_MODEL, IN_WIDTH), D_MODEL),
        "b_gate": 0.01 * jax.random.normal(ks[3], (L, N_BRANCHES * D_MODEL), f32),
        "pool_w": nrm(ks[4], (L, N_POOL_GROUPS, POOL_GROUP, POOL_GROUP), POOL_GROUP),
        "pool_scale": gain(ks[5], (L, POOL_WIDTH)),
        "w_branch_pool": nrm(ks[6], (L, POOL_WIDTH, D_MODEL), POOL_WIDTH),
        "w_branch_attn": nrm(ks[7], (L, ATTN_WIDTH, D_MODEL), ATTN_WIDTH),
        "w_out": nrm(ks[8], (L, D_MODEL, D_MODEL), D_MODEL),
        "norm_mix_post": gain(ks[9], (L, D_MODEL)),
        "norm_mlp_pre": gain(ks[10], (L, D_MODEL)),
        "w_up": nrm(ks[11], (L, D_MODEL, D_FF), D_MODEL),
        "w_down": nrm(ks[12], (L, D_FF, D_MODEL), D_FF),
        "norm_mlp_post": gain(ks[13], (L, D_MODEL)),
    }


def reference(x, norm_mix_pre, w_in, b_gate, pool_w, pool_scale, w_branch_pool, w_branch_attn, w_out,
              norm_mix_post, norm_mlp_pre, w_up, w_down, norm_mlp_post):
    B, S, _ = x.shape
    h = x
    o_q = POOL_WIDTH
    o_k = o_q + ATTN_WIDTH
    o_v = o_k + ATTN_WIDTH
    o_g = o_v + ATTN_WIDTH
    for l in range(DEPTH):
        u = rmsnorm(h, norm_mix_pre[l])
        z = u @ w_in[l]
        p = z[..., :o_q]
        q = z[..., o_q:o_k].reshape(B, S, N_HEADS, HEAD_DIM)
        k = z[..., o_k:o_v].reshape(B, S, N_HEADS, HEAD_DIM)
        v = z[..., o_v:o_g].reshape(B, S, N_HEADS, HEAD_DIM)
        g = z[..., o_g:] + b_gate[l]
        g_pool, g_attn = g[..., :D_MODEL], g[..., D_MODEL:]
        y_pool = pool_mixer(p, pool_w[l], pool_scale[l]) @ w_branch_pool[l]
        y_attn = moba_attention(q, k, v) @ w_branch_attn[l]
        m = jax.nn.sigmoid(g_pool) * y_pool + jax.nn.sigmoid(g_attn) * y_attn
        h = h + rmsnorm(m @ w_out[l], norm_mix_post[l])
        u2 = rmsnorm(h, norm_mlp_pre[l])
        a = jnp.square(jax.nn.relu(u2 @ w_up[l]))
        h = h + rmsnorm(a @ w_down[l], norm_mlp_post[l])
    return h
```

```python
import contextlib
import numpy as np
import ml_dtypes
import concourse.bass as bass
import concourse.mybir as mybir
from concourse.bass_utils import run_bass_kernel_spmd

F32 = mybir.dt.float32
BF16 = mybir.dt.bfloat16
AF = mybir.ActivationFunctionType
ALU = mybir.AluOpType
AX = mybir.AxisListType

COMPUTE = ("pe", "act", "dve", "pool")

D = 1024
S = 2048
NSEQ = 2
H = 8
DH = 64
NB = 8
G = 512
NG = S // G
EPS = 1e-6
NEG = -30000.0
NSLOT = 4
SLOT = 8192


class V:
    __slots__ = ("ap", "key", "lo", "hi")

    def __init__(self, ap, key, lo, hi):
        self.ap, self.key, self.lo, self.hi = ap, key, lo, hi

    def re(self, pat, **kw):
        return V(self.ap.rearrange(pat, **kw), self.key, self.lo, self.hi)

    def __getitem__(self, idx):
        return V(self.ap[idx], self.key, self.lo, self.hi)

    def bc(self, shape):
        return V(self.ap.to_broadcast(shape), self.key, self.lo, self.hi)


class Buf:
    def __init__(self, nc, name, nbytes, psum=False):
        self.name = name
        self.nbytes = nbytes
        self.psum = psum
        if psum:
            self.t = nc.alloc_psum_tensor(name, [128, nbytes // 4], F32)
        else:
            self.t = nc.alloc_sbuf_tensor(name, [128, nbytes // 2], BF16)

    def v(self, lo, hi, dt=BF16):
        if self.psum:
            ap = self.t[:, lo // 4:hi // 4]
            if dt != F32:
                ap = ap.bitcast(dt)
        else:
            ap = self.t[:, lo // 2:hi // 2]
            if dt != BF16:
                ap = ap.bitcast(dt)
        return V(ap, self.name, lo, hi)


class Prog:
    def __init__(self, nc):
        self.nc = nc
        self.ops = {e: [] for e in COMPUTE + ("sp",)}
        self.count = {e: 0 for e in COMPUTE}
        self.seen = {e: {} for e in COMPUTE + ("sp",)}
        self.hist = {}
        self.known = {}
        self.dma_count = {}
        self.sem_names = set(COMPUTE)

    def _deps(self, eng, reads, writes):
        deps = {}
        for r in reads:
            for (lo, hi, kind, tok) in self.hist.get(r.key, ()):
                if kind == "w" and lo < r.hi and r.lo < hi:
                    if deps.get(tok[0], 0) < tok[1]:
                        deps[tok[0]] = tok[1]
        for w in writes:
            for (lo, hi, kind, tok) in self.hist.get(w.key, ()):
                if lo < w.hi and w.lo < hi:
                    if deps.get(tok[0], 0) < tok[1]:
                        deps[tok[0]] = tok[1]
        waits = []
        seen = self.seen[eng]
        for sem, val in deps.items():
            if sem == eng and eng == "pe":
                continue
            if seen.get(sem, 0) >= val:
                continue
            waits.append((sem, val))
        for sem, val in waits:
            if seen.get(sem, 0) < val:
                seen[sem] = val
            kn = self.known.get((sem, val))
            if kn:
                for s2, v2 in kn.items():
                    if seen.get(s2, 0) < v2:
                        seen[s2] = v2
        return waits

    def _record(self, tok, reads, writes):
        for w in writes:
            lst = self.hist.setdefault(w.key, [])
            lst[:] = [rec for rec in lst if not (w.lo <= rec[0] and rec[1] <= w.hi)]
            lst.append((w.lo, w.hi, "w", tok))
        for r in reads:
            lst = self.hist.setdefault(r.key, [])
            lst[:] = [rec for rec in lst if not (rec[2] == "r" and rec[3][0] == tok[0]
                                                  and r.lo <= rec[0] and rec[1] <= r.hi)]
            lst.append((r.lo, r.hi, "r", tok))

    def op(self, eng, fn, reads=(), writes=()):
        reads = [r for r in reads if isinstance(r, V)]
        writes = [w for w in writes if isinstance(w, V)]
        waits = self._deps(eng, reads, writes)
        self.count[eng] += 1
        tok = (eng, self.count[eng])
        self.known[tok] = dict(self.seen[eng])
        self._record(tok, reads, writes)
        self.ops[eng].append((waits, fn, (eng, 1)))
        return tok

    def dma(self, sem, out, in_, queue="sp"):
        self.sem_names.add(sem)
        reads = [in_] if isinstance(in_, V) else []
        writes = [out] if isinstance(out, V) else []
        waits = self._deps(queue, reads, writes)
        self.dma_count[sem] = self.dma_count.get(sem, 0) + 1
        tok = (sem, 16 * self.dma_count[sem])
        self.known[tok] = dict(self.seen[queue])
        self._record(tok, reads, writes)
        o = out.ap if isinstance(out, V) else out
        i = in_.ap if isinstance(in_, V) else in_
        self.ops[queue].append((waits, lambda e: e.dma_start(out=o, in_=i), (sem, 16)))
        return tok

    def final_wait(self, eng="sp"):
        waits = []
        for sem, cnt in self.dma_count.items():
            if self.seen[eng].get(sem, 0) < 16 * cnt:
                waits.append((sem, 16 * cnt))
        self.ops[eng].append((waits, None, None))

    def mm(self, out, lhsT, rhs, start=True, stop=True):
        return self.op("pe", lambda e: e.matmul(out.ap, lhsT=lhsT.ap, rhs=rhs.ap, start=start, stop=stop),
                       reads=[lhsT, rhs], writes=[out])

    def tr(self, out, in_, ident):
        return self.op("pe", lambda e: e.transpose(out=out.ap, in_=in_.ap, identity=ident.ap),
                       reads=[in_, ident], writes=[out])

    def act(self, out, in_, func, bias=None, scale=None, accum_out=None):
        kw = {}
        rd = [in_]
        wr = [out]
        if bias is not None:
            kw["bias"] = bias.ap if isinstance(bias, V) else bias
            rd.append(bias)
        if scale is not None:
            kw["scale"] = scale.ap if isinstance(scale, V) else scale
            rd.append(scale)
        if accum_out is not None:
            kw["accum_out"] = accum_out.ap
            wr.append(accum_out)
        return self.op("act", lambda e: e.activation(out=out.ap, in_=in_.ap, func=func, **kw), reads=rd, writes=wr)

    def copy(self, eng, out, in_):
        if eng == "act":
            return self.act(out, in_, AF.Copy)
        return self.op(eng, lambda e: e.tensor_copy(out=out.ap, in_=in_.ap), reads=[in_], writes=[out])

    def tt(self, eng, out, in0, in1, op):
        return self.op(eng, lambda e: e.tensor_tensor(out=out.ap, in0=in0.ap, in1=in1.ap, op=op),
                       reads=[in0, in1], writes=[out])

    def ts(self, eng, out, in0, s1, op0, s2=None, op1=None):
        a1 = s1.ap if isinstance(s1, V) else s1
        a2 = s2.ap if isinstance(s2, V) else s2
        kw = {}
        if op1 is not None:
            kw["op1"] = op1
        return self.op(eng, lambda e: e.tensor_scalar(out=out.ap, in0=in0.ap, scalar1=a1, scalar2=a2, op0=op0, **kw),
                       reads=[in0, s1, s2], writes=[out])

    def stt(self, eng, out, in0, scalar, in1, op0, op1):
        a = scalar.ap if isinstance(scalar, V) else scalar
        return self.op(eng, lambda e: e.scalar_tensor_tensor(out=out.ap, in0=in0.ap, scalar=a, in1=in1.ap,
                                                             op0=op0, op1=op1),
                       reads=[in0, scalar, in1], writes=[out])

    def reduce(self, eng, out, in_, op, axis=AX.X):
        return self.op(eng, lambda e: e.tensor_reduce(out=out.ap, in_=in_.ap, axis=axis, op=op),
                       reads=[in_], writes=[out])

    def recip(self, out, in_):
        return self.op("dve", lambda e: e.reciprocal(out=out.ap, in_=in_.ap), reads=[in_], writes=[out])

    def memset(self, eng, out, val):
        return self.op(eng, lambda e: e.memset(out.ap, val), writes=[out])

    def emit(self):
        nc = self.nc
        names = sorted(self.sem_names)
        sems = {}
        with contextlib.ExitStack() as st:
            for n in names:
                sems[n] = st.enter_context(nc.semaphore("s_" + n))
            block = st.enter_context(nc.Block())

            def run(eng_name):
                def body(e):
                    for waits, fn, inc in self.ops[eng_name]:
                        for sem, val in waits:
                            e.wait_ge(sems[sem], val)
                        if fn is not None:
                            fn(e).then_inc(sems[inc[0]], inc[1])
                return body

            block.sync(run("sp"))
            block.tensor(run("pe"))
            block.scalar(run("act"))
            block.vector(run("dve"))
            block.gpsimd(run("pool"))


def _consts():
    bf = ml_dtypes.bfloat16
    ident = np.eye(128, dtype=np.float32).astype(bf)
    tri = (np.arange(128)[None, :] >= np.arange(128)[:, None]).astype(np.float32).astype(bf)
    slopes = 2.0 ** (-8.0 * np.arange(1, H + 1) / H)
    t = np.arange(S)
    a, b = t // 256, t % 256
    kaug = np.zeros((32, H, S), np.float32)
    qaug = np.zeros((S, H, 96), np.float32)
    for h in range(H):
        sl = slopes[h]
        kaug[0, h] = 1.0
        kaug[1, h] = 1.0
        kaug[2, h] = sl * b
        kaug[3, h] = sl * 256.0 * a
        for n in range(NB):
            kaug[4 + n, h] = (a == n)
        qaug[:, h, 64] = -sl * b
        qaug[:, h, 65] = -sl * 256.0 * a
        qaug[:, h, 66] = 1.0
        qaug[:, h, 67] = 1.0
        for n in range(NB):
            allowed = (n == a) | ((n < a) & (a <= 3))
            qaug[:, h, 68 + n] = np.where(allowed, 0.0, NEG)
    qaug = qaug.reshape(S // 128, 128, H * 96)
    rcfix = np.broadcast_to(1.0 / (np.arange(16) + 1.0), (128, 16)).astype(np.float32)
    return dict(c_ident=ident, c_tri=tri, c_kaug=kaug.reshape(32, H * S).astype(bf),
                c_qaug=qaug.astype(bf), c_rcfix=np.ascontiguousarray(rcfix))


def build_nc():
    nc = bass.Bass("TRN2", target_bir_lowering=False)
    T = NSEQ * S

    def din(name, shape, dt=F32):
        return nc.dram_tensor(name, list(shape), dt, kind="ExternalInput").ap()

    x = din("x", [T, D])
    w_in = din("w_in", [D, 4096])
    w_bp = din("w_bp", [512, D])
    w_ba = din("w_ba", [512, D])
    w_out = din("w_out", [D, D])
    w_up = din("w_up", [D, 4096])
    w_down = din("w_down", [4096, D])
    pool_w = din("pool_w", [512, 128])
    g_all = din("g_all", [4, D])
    smallv = din("smallv", [128, 20])
    c_ident = din("c_ident", [128, 128], BF16)
    c_tri = din("c_tri", [128, 128], BF16)
    c_kaug = din("c_kaug", [32, H * S], BF16)
    c_qaug = din("c_qaug", [S // 128, 128, H * 96], BF16)
    c_rcfix = din("c_rcfix", [128, 16])
    out = nc.dram_tensor("out", [T, D], F32, kind="ExternalOutput").ap()

    def scr(name, shape):
        return nc.dram_tensor(name, list(shape), BF16, kind="Internal").ap()

    s_win = scr("s_win", [D, 2048])
    s_wg = scr("s_wg", [D, 2048])
    s_wmix = scr("s_wmix", [512, 2048])
    s_wout = scr("s_wout", [D, D])
    s_wup = scr("s_wup", [D, 4096])
    s_wdown = scr("s_wdown", [4096, D])
    s_pw = scr("s_pw", [512, 128])

    p = Prog(nc)

    b_ident = Buf(nc, "ident", 256)
    b_tri = Buf(nc, "tri", 256)
    b_g = Buf(nc, "gbc", 4 * 4096)
    b_small = Buf(nc, "small", 1024)
    b_pw = Buf(nc, "poolw", 1024)
    b_stats = Buf(nc, "stats", 512)
    b_ring = Buf(nc, "ring", NSLOT * SLOT)
    b_pmT = Buf(nc, "pmT", 16384)
    b_attnT = Buf(nc, "attnT", 16384)
    ARENA = 126976
    b_ar = Buf(nc, "arena", ARENA)
    PS = [Buf(nc, f"ps{i}", 2048, psum=True) for i in range(8)]

    ident = b_ident.v(0, 256)
    tri = b_tri.v(0, 256)
    gbc = [b_g.v(i * 4096, (i + 1) * 4096, F32) for i in range(4)]
    bgate = b_small.v(0, 64, F32)
    pscale = b_small.v(64, 80, F32)
    rcfix = b_small.v(128, 192, F32)
    poolw = b_pw.v(0, 1024).re("p (g c) -> p g c", g=4)
    pmT = b_pmT.v(0, 16384).re("p (g t) -> p g t", g=4)
    attnT = b_attnT.v(0, 16384).re("p (i t) -> p i t", i=4)

    QA, KA, VA, WA = 0, 32768, 65536, 90112
    q_aug = b_ar.v(QA, QA + 32768).re("p (h t) -> p h t", h=H)
    k_aug = b_ar.v(KA, KA + 32768).re("p (h t) -> p h t", h=H)
    v_aug = b_ar.v(VA, VA + 24576).re("p (k c) -> p k c", k=16)

    def wa(lo, hi, dt=BF16):
        return b_ar.v(WA + lo, WA + hi, dt)

    stat_i = [0]

    def stat():
        i = stat_i[0] % 16
        stat_i[0] += 1
        return b_stats.v(i * 32, i * 32 + 32, F32)

    p.dma("c0", ident, c_ident)
    p.dma("c0", tri, c_tri)
    for i in range(4):
        p.dma("c0", gbc[i], g_all[i].partition_broadcast(128))
    p.dma("c0", b_small.v(0, 80, F32), smallv)
    p.dma("c0", rcfix, c_rcfix)

    def scr_v(ap, key):
        return V(ap, key, 0, 1)

    def cast(key, dst, src, rows):
        n = dst.shape[0]
        for r0 in range(0, n, rows):
            p.dma("k_" + key, scr_v(dst[r0:r0 + rows], key), src[r0:r0 + rows], queue="pool")

    cast("win", s_win, w_in[:, 0:2048], 256)
    cast("pw", s_pw, pool_w, 512)
    for r in range(4):
        p.dma("k_wg", scr_v(s_wg[:, 512 * r:512 * r + 256], "wg"), w_in[:, 2048 + 256 * r:2048 + 256 * r + 256], queue="pool")
        p.dma("k_wg", scr_v(s_wg[:, 512 * r + 256:512 * r + 512], "wg"), w_in[:, 3072 + 256 * r:3072 + 256 * r + 256], queue="pool")
    for hh in range(2):
        p.dma("k_wmix", scr_v(s_wmix[:, 1024 * hh:1024 * hh + 512], "wmix"), w_bp[:, 512 * hh:512 * hh + 512], queue="pool")
        p.dma("k_wmix", scr_v(s_wmix[:, 1024 * hh + 512:1024 * hh + 1024], "wmix"), w_ba[:, 512 * hh:512 * hh + 512], queue="pool")
    cast("wout", s_wout, w_out, 256)
    cast("wup", s_wup, w_up, 256)
    cast("wdown", s_wdown, w_down, 512)
    p.dma("c0", b_pw.v(0, 1024), scr_v(s_pw.rearrange("(g p) c -> p g c", p=128), "pw"))

    def piece_src(kind, idx):
        if kind == "win":
            return scr_v(s_win.rearrange("(kc p) n -> p kc n", p=128)[:, :, 512 * idx:512 * idx + 512], "win"), 8, 512
        if kind == "wg":
            return scr_v(s_wg.rearrange("(kc p) n -> p kc n", p=128)[:, :, 512 * idx:512 * idx + 512], "wg"), 8, 512
        if kind == "wmix":
            return scr_v(s_wmix.rearrange("(kc p) n -> p kc n", p=128)[:, :, 1024 * idx:1024 * idx + 1024], "wmix"), 4, 1024
        if kind == "wout":
            return scr_v(s_wout.rearrange("(kc p) n -> p kc n", p=128)[:, :, 512 * idx:512 * idx + 512], "wout"), 8, 512
        if kind == "wup":
            return scr_v(s_wup.rearrange("(kc p) n -> p kc n", p=128)[:, :, 512 * idx:512 * idx + 512], "wup"), 8, 512
        if kind == "wdown":
            half, kg = idx // 4, idx % 4
            return scr_v(s_wdown.rearrange("(kc p) n -> p kc n", p=128)[:, 8 * kg:8 * kg + 8, 512 * half:512 * half + 512],
                         "wdown"), 8, 512
        raise KeyError(kind)

    pieces = []
    for s in range(NSEQ):
        for g in range(NG):
            pieces += [("A", s, g, "win", i) for i in range(4)]
        for g in range(NG):
            pieces += [("C", s, g, "wmix", 0), ("C", s, g, "wg", 0), ("C", s, g, "wg", 1),
                       ("C", s, g, "wmix", 1), ("C", s, g, "wg", 2), ("C", s, g, "wg", 3)]
            pieces += [("C", s, g, "wout", i) for i in range(2)]
            pieces += [("C", s, g, "wup", i) for i in range(8)]
            pieces += [("C", s, g, "wdown", i) for i in range(8)]
    piece_idx = {pc: i for i, pc in enumerate(pieces)}
    ring = dict(next=0, free=list(range(NSLOT)), loaded={})

    def ring_fill():
        while ring["free"] and ring["next"] < len(pieces):
            slot = ring["free"].pop(0)
            i = ring["next"]
            ring["next"] += 1
            src, kc, n = piece_src(pieces[i][3], pieces[i][4])
            dst = b_ring.v(slot * SLOT, (slot + 1) * SLOT).re("p (kc n) -> p kc n", kc=kc)
            p.dma(f"ring{slot}", dst, src)
            ring["loaded"][i] = (slot, dst)

    def ring_get(pc):
        i = piece_idx[pc]
        ring_fill()
        assert i in ring["loaded"], ("ring piece not loaded (deadlock in schedule)", pc, ring)
        return ring["loaded"][i][1]

    def ring_rel(pc):
        i = piece_idx[pc]
        slot, _ = ring["loaded"].pop(i)
        ring["free"].append(slot)
        ring_fill()

    evac_rr = [0]

    def evac_eng():
        evac_rr[0] += 1
        return "act" if evac_rr[0] % 2 else "dve"

    def rmsnorm_rstd(src_views, st):
        junk_views = rmsnorm_rstd.junk
        p.memset("dve", st[:, 0:4], 0.0)
        for i, sv in enumerate(src_views):
            p.act(junk_views[i], sv, AF.Square, accum_out=st[:, i:i + 1])
        if len(src_views) == 2:
            p.tt("dve", st[:, 0:1], st[:, 0:1], st[:, 1:2], ALU.add)
        p.act(st[:, 4:5], st[:, 0:1], AF.Sqrt, scale=1.0 / D, bias=EPS)
        p.recip(st[:, 5:6], st[:, 4:5])
        return st[:, 5:6]

    def transpose_tile(xn_v, dstT, tt, bank):
        pst = bank.v(0, 2048, BF16)
        for kc in range(8):
            p.tr(pst[:, kc * 128:(kc + 1) * 128], xn_v[:, kc * 128:(kc + 1) * 128], ident)
        p.copy(evac_eng(), dstT[:, :, tt * 128:(tt + 1) * 128], pst.re("p (kc t) -> p kc t", kc=8))

    for s in range(NSEQ):
        tok0 = s * S
        p.dma("c1", k_aug[64:96], c_kaug.rearrange("p (h t) -> p h t", h=H))
        ones_v = v_aug.re("p k (i b c) -> p k i b c", i=4, b=3)[:, :, :, 1, :]
        p.memset("pool", ones_v, 1.0)

        xa = [wa(0, 4096, F32), wa(4096, 8192, F32)]
        xn = [wa(8192, 10240), wa(10240, 12288)]
        uT = wa(12288, 20480).re("p (kc t) -> p kc t", kc=8)
        P4 = [wa(20480 + i * 2112, 20480 + (i + 1) * 2112, F32) for i in range(4)]
        T0 = wa(28928, 31040, F32)
        T1 = wa(31040, 33152, F32)
        yb = [wa(33152, 34176), wa(34176, 35200)]
        tile_ctr = 0
        pending_pw = []

        def flush_pw():
            while pending_pw:
                gg, grp, ybv = pending_pw.pop(0)
                pb = PS[7].v(0, 2048, F32)
                p.mm(pb, poolw[:, grp, :], ybv)
                p.act(pmT[:, grp, gg * G:(gg + 1) * G], pb, AF.Copy, scale=pscale[:, grp:grp + 1])

        for g in range(NG):
            for grp in range(4):
                if g == 0:
                    p.memset("pool", P4[grp][:, 0:16], 0.0)
                else:
                    p.copy("pool", P4[grp][:, 0:16], P4[grp][:, 512:528])
            for tt in range(4):
                xv = xa[tile_ctr % 2]
                xnv = xn[tile_ctr % 2]
                tile_ctr += 1
                r0 = tok0 + g * G + tt * 128
                p.dma(f"xa{tile_ctr % 2}", xv, x[r0:r0 + 128, :])
                rmsnorm_rstd.junk = [xnv]
                rstd = rmsnorm_rstd([xv], stat())
                p.stt("dve", xnv, xv, rstd, gbc[0], ALU.mult, ALU.mult)
                transpose_tile(xnv, uT, tt, PS[6])
            flush_pw()
            for ci in range(12):
                kind = ci // 4
                sub = ci % 4
                pc = ("A", s, g, "win", kind)
                w = ring_get(pc)
                bank = PS[ci % 4].v(0, 2048, F32)
                for kc in range(8):
                    p.mm(bank, w[:, kc, sub * 128:(sub + 1) * 128], uT[:, kc, :], start=(kc == 0), stop=(kc == 7))
                cols = slice(g * G, (g + 1) * G)
                if kind == 0:
                    p.copy(evac_eng(), P4[sub][:, 16:528], bank)
                elif kind == 1:
                    p.act(q_aug[0:64, 2 * sub, cols], bank[0:64], AF.Copy, scale=0.125)
                    p.ts("dve", q_aug[0:64, 2 * sub + 1, cols], bank[64:128], 0.125, ALU.mult)
                else:
                    p.copy("act", k_aug[0:64, 2 * sub, cols], bank[0:64])
                    p.copy("dve", k_aug[0:64, 2 * sub + 1, cols], bank[64:128])
                if sub == 3:
                    ring_rel(pc)
            pc = ("A", s, g, "win", 3)
            w = ring_get(pc)
            for tt in range(4):
                bank = PS[4 + tt % 2].v(0, 2048, F32)
                for kc in range(8):
                    p.mm(bank, uT[:, kc, tt * 128:(tt + 1) * 128], w[:, kc, :], start=(kc == 0), stop=(kc == 7))
                kt = g * 4 + tt
                dst = v_aug[:, kt, :].re("p (i b c) -> p i b c", i=4, b=3)
                src = bank.re("p (i b c) -> p i b c", i=4, b=2)
                p.copy("act", dst[:, :, 0, :], src[:, :, 0, :])
                p.copy("dve", dst[:, :, 2, :], src[:, :, 1, :])
            ring_rel(pc)
            for grp in range(4):
                Pg = P4[grp]
                w_ = 2 ** (grp + 1)
                p.tt("pool", T0[:, 1:528], Pg[:, 1:528], Pg[:, 0:527], ALU.add)
                last = T0
                if grp >= 1:
                    p.tt("pool", T1[:, 3:528], T0[:, 3:528], T0[:, 1:526], ALU.add)
                    last = T1
                if grp >= 2:
                    p.tt("pool", T0[:, 7:528], T1[:, 7:528], T1[:, 3:524], ALU.add)
                    last = T0
                if grp >= 3:
                    p.tt("pool", T1[:, 15:528], T0[:, 15:528], T0[:, 7:520], ALU.add)
                    last = T1
                ybv = yb[grp % 2]
                p.stt("dve", ybv, last[:, 16:528], 1.0 / w_, Pg[:, 16:528], ALU.mult, ALU.subtract)
                if g == 0:
                    n = w_ - 1
                    p.tt("pool", last[:, 16:16 + n], last[:, 16:16 + n], rcfix[:, 0:n], ALU.mult)
                    p.tt("pool", ybv[:, 0:n], last[:, 16:16 + n], Pg[:, 16:16 + n], ALU.subtract)
                pending_pw.append((g, grp, ybv))
                if grp % 2 == 1:
                    flush_pw()
        flush_pw()

        pt = [wa(i * 1024, (i + 1) * 1024) for i in range(4)]
        rden = [wa(4096 + i * 2048, 4096 + (i + 1) * 2048, F32) for i in range(2)]
        mbq = [wa(8192 + i * 1536, 8192 + (i + 1) * 1536).re("p (h c) -> p h c", h=H) for i in range(2)]
        g_sb = wa(11264, 11520, F32).re("p (h n) -> p h n", h=H)
        kms = wa(13312, 13568, F32)
        kmT = wa(13568, 13696)

        p.reduce("dve", kms[0:64], k_aug[0:64].re("p h (n j) -> p (h n) j", n=NB), ALU.add)
        p.ts("dve", kmT[0:64], kms[0:64], 1.0 / 256, ALU.mult)
        for qt in range(S // 128):
            qb = qt // 2
            mb = mbq[qt % 2]
            p.dma(f"mb{qt % 2}", mb, c_qaug[qt].rearrange("p (h c) -> p h c", h=H))
            if qb >= 4:
                npast = qb
                psG = PS[6].v(0, 256, F32)
                for h in range(H):
                    p.mm(psG[:, h * 8:(h + 1) * 8], q_aug[0:64, h, qt * 128:(qt + 1) * 128], kmT[0:64, h * 8:(h + 1) * 8])
                p.copy("dve", g_sb, psG.re("p (h n) -> p h n", h=H))
                gt = g_sb[:, :, 0:npast]
                cmpb = wa(11520, 11520 + H * npast * npast * 4, F32).re("p (h n m) -> p h n m", h=H, n=npast)
                in0 = V(gt.ap.unsqueeze(2).to_broadcast([128, H, npast, npast]), gt.key, gt.lo, gt.hi)
                in1 = V(gt.ap.unsqueeze(3).to_broadcast([128, H, npast, npast]), gt.key, gt.lo, gt.hi)
                p.tt("dve", cmpb, in0, in1, ALU.is_gt)
                rank = wa(13088, 13088 + H * npast * 4, F32).re("p (h n) -> p h n", h=H)
                p.reduce("dve", rank, cmpb, ALU.add)
                p.ts("dve", mb[:, :, 68:68 + npast], rank, 2.5, ALU.is_gt, NEG, ALU.mult)
            psT = PS[7].v(0, 2048, BF16)
            for h in range(H):
                p.tr(psT[0:96, h * 128:(h + 1) * 128], mb[:, h, :], ident)
            p.copy("act", q_aug[64:96, :, qt * 128:(qt + 1) * 128], psT[64:96].re("p (h q) -> p h q", h=H))

        tiles = []
        for i in range(4):
            for j in range(4):
                for e in range(2):
                    h = 2 * i + e
                    nk = 4 * j + 4
                    for kt in range(nk):
                        c0 = 0 if kt < 4 * j else 128 * (kt - 4 * j)
                        tiles.append((i, j, e, h, kt, c0, nk))
        LOOK = 3
        acc_ctr = [0]

        def emit_score(n):
            i, j, e, h, kt, c0, nk = tiles[n]
            sb = PS[n % 4].v(0, 2048, F32)
            p.mm(sb[:, c0:512], k_aug[0:96, h, kt * 128:(kt + 1) * 128], q_aug[0:96, h, j * 512 + c0:(j + 1) * 512])
            ptv = pt[n % 4]
            p.act(ptv[:, c0:512], sb[:, c0:512], AF.Exp)
            if kt >= 4 * j:
                p.tt("pool", ptv[:, c0:c0 + 128], ptv[:, c0:c0 + 128], tri, ALU.mult)

        def emit_pv(n):
            i, j, e, h, kt, c0, nk = tiles[n]
            if kt == 0:
                acc_ctr[0] += 1
            ob = PS[4 + acc_ctr[0] % 2].v(0, 2048, F32)
            lo = 192 * i + 64 * e
            p.mm(ob[:, c0:512], v_aug[:, kt, lo:lo + 128], pt[n % 4][:, c0:512], start=(kt == 0), stop=(kt == nk - 1))
            if kt == nk - 1:
                cols = slice(j * 512, (j + 1) * 512)
                rd = rden[acc_ctr[0] % 2]
                if e == 0:
                    p.recip(rd[0:64], ob[64:128])
                    p.tt("dve", attnT[0:64, i, cols], ob[0:64], rd[0:64], ALU.mult)
                else:
                    p.recip(rd[64:128], ob[0:64])
                    p.tt("dve", attnT[64:128, i, cols], ob[64:128], rd[64:128], ALU.mult)

        for n in range(len(tiles) + LOOK):
            if n < len(tiles):
                emit_score(n)
            if n >= LOOK:
                emit_pv(n - LOOK)

        def ar(lo, hi, dt=BF16):
            return b_ar.v(lo, hi, dt)

        xh = [ar(i * 4096, (i + 1) * 4096, F32) for i in range(4)]
        xn2 = [ar(16384 + i * 2048, 16384 + (i + 1) * 2048) for i in range(2)]
        uT2 = ar(20480, 28672).re("p (kc t) -> p kc t", kc=8)
        sg = [ar(28672 + i * 2048, 28672 + (i + 1) * 2048, F32) for i in range(4)]
        t12 = [ar(36864 + i * 2048, 36864 + (i + 1) * 2048, F32) for i in range(4)]
        mT = ar(45056, 53248).re("p (kc t) -> p kc t", kc=8)
        u2T = ar(53248, 61440).re("p (kc t) -> p kc t", kc=8)
        aT = ar(61440, 94208).re("p (f t) -> p f t", f=32)
        tmpn = [ar(94208 + i * 4096, 94208 + (i + 1) * 4096, F32) for i in range(2)]
        ot = [ar(102400 + i * 4096, 102400 + (i + 1) * 4096, F32) for i in range(2)]
        rl = [ar(110592 + i * 2048, 110592 + (i + 1) * 2048, F32) for i in range(4)]
        octr = 0

        for g in range(NG):
            cols = slice(g * G, (g + 1) * G)
            for tt in range(4):
                r0 = tok0 + g * G + tt * 128
                p.dma(f"xc{tt}", xh[tt], x[r0:r0 + 128, :])
                xnv = xn2[tt % 2]
                rmsnorm_rstd.junk = [xnv]
                rstd = rmsnorm_rstd([xh[tt]], stat())
                p.stt("dve", xnv, xh[tt], rstd, gbc[0], ALU.mult, ALU.mult)
                transpose_tile(xnv, uT2, tt, PS[7])
            for c in range(8):
                hh = c // 4
                pmix = ("C", s, g, "wmix", hh)
                wmix = ring_get(pmix)
                pg_ = ("C", s, g, "wg", c // 2)
                wg = ring_get(pg_)
                base = 0 if c % 2 == 0 else 4
                byp = PS[base + 0].v(0, 2048, F32)
                bya = PS[base + 1].v(0, 2048, F32)
                bgp = PS[base + 2].v(0, 2048, F32)
                bga = PS[base + 3].v(0, 2048, F32)
                cc = (c % 4) * 128
                for kc in range(4):
                    p.mm(byp, wmix[:, kc, cc:cc + 128], pmT[:, kc, cols], start=(kc == 0), stop=(kc == 3))
                for kc in range(4):
                    p.mm(bya, wmix[:, kc, 512 + cc:512 + cc + 128], attnT[:, kc, cols], start=(kc == 0), stop=(kc == 3))
                gc = (c % 2) * 128
                for kc in range(8):
                    p.mm(bgp, wg[:, kc, gc:gc + 128], uT2[:, kc, :], start=(kc == 0), stop=(kc == 7))
                for kc in range(8):
                    p.mm(bga, wg[:, kc, 256 + gc:256 + gc + 128], uT2[:, kc, :], start=(kc == 0), stop=(kc == 7))
                s0, s1 = sg[(c % 2) * 2], sg[(c % 2) * 2 + 1]
                t0, t1 = t12[(c % 2) * 2], t12[(c % 2) * 2 + 1]
                p.act(s0, bgp, AF.Sigmoid, bias=bgate[:, c:c + 1])
                p.act(s1, bga, AF.Sigmoid, bias=bgate[:, 8 + c:9 + c])
                p.tt("dve", t0, byp, s0, ALU.mult)
                p.tt("dve", t1, bya, s1, ALU.mult)
                p.tt("pool", mT[:, c, :], t0, t1, ALU.add)
                if c % 2 == 1:
                    ring_rel(pg_)
                if c % 4 == 3:
                    ring_rel(pmix)
            for half in range(2):
                pc = ("C", s, g, "wout", half)
                w = ring_get(pc)
                for kc in range(8):
                    for tt in range(4):
                        bank = PS[half * 4 + tt].v(0, 2048, F32)
                        p.mm(bank, mT[:, kc, tt * 128:(tt + 1) * 128], w[:, kc, :], start=(kc == 0), stop=(kc == 7))
                ring_rel(pc)
            for tt in range(4):
                b0 = PS[tt].v(0, 2048, F32)
                b1 = PS[4 + tt].v(0, 2048, F32)
                tv = tmpn[tt % 2]
                rmsnorm_rstd.junk = [xn2[tt % 2][:, 0:512], xn2[tt % 2][:, 512:1024]]
                rstd = rmsnorm_rstd([b0, b1], stat())
                p.stt("dve", tv[:, 0:512], b0, rstd, gbc[1][:, 0:512], ALU.mult, ALU.mult)
                p.stt("dve", tv[:, 512:1024], b1, rstd, gbc[1][:, 512:1024], ALU.mult, ALU.mult)
                p.tt("pool", xh[tt], xh[tt], tv, ALU.add)
                xnv = xn2[tt % 2]
                rmsnorm_rstd.junk = [xnv]
                rstd2 = rmsnorm_rstd([xh[tt]], stat())
                p.stt("dve", xnv, xh[tt], rstd2, gbc[2], ALU.mult, ALU.mult)
                transpose_tile(xnv, u2T, tt, PS[tt])
            for f in range(32):
                pc = ("C", s, g, "wup", f // 4)
                w = ring_get(pc)
                bank = PS[4 + f % 4].v(0, 2048, F32)
                fc = (f % 4) * 128
                for kc in range(8):
                    p.mm(bank, w[:, kc, fc:fc + 128], u2T[:, kc, :], start=(kc == 0), stop=(kc == 7))
                rv = rl[f % 4]
                p.act(rv, bank, AF.Relu)
                p.tt("pool" if f % 2 else "dve", aT[:, f, :], rv, rv, ALU.mult)
                if f % 4 == 3:
                    ring_rel(pc)
            for half in range(2):
                for kg in range(4):
                    pc = ("C", s, g, "wdown", half * 4 + kg)
                    w = ring_get(pc)
                    for k8 in range(8):
                        f = kg * 8 + k8
                        for tt in range(4):
                            bank = PS[half * 4 + tt].v(0, 2048, F32)
                            p.mm(bank, aT[:, f, tt * 128:(tt + 1) * 128], w[:, k8, :], start=(f == 0), stop=(f == 31))
                    ring_rel(pc)
            for tt in range(4):
                b0 = PS[tt].v(0, 2048, F32)
                b1 = PS[4 + tt].v(0, 2048, F32)
                tv = tmpn[tt % 2]
                rmsnorm_rstd.junk = [xn2[tt % 2][:, 0:512], xn2[tt % 2][:, 512:1024]]
                rstd = rmsnorm_rstd([b0, b1], stat())
                p.stt("dve", tv[:, 0:512], b0, rstd, gbc[3][:, 0:512], ALU.mult, ALU.mult)
                p.stt("dve", tv[:, 512:1024], b1, rstd, gbc[3][:, 512:1024], ALU.mult, ALU.mult)
                ov = ot[octr % 2]
                p.tt("pool", ov, xh[tt], tv, ALU.add)
                r0 = tok0 + g * G + tt * 128
                p.dma(f"st{octr % 2}", out[r0:r0 + 128, :], ov)
                octr += 1

    p.final_wait("sp")
    p.emit()
    return nc, p


_CACHE = {}


def kernel(x, norm_mix_pre, w_in, b_gate, pool_w, pool_scale, w_branch_pool, w_branch_attn, w_out,
           norm_mix_post, norm_mlp_pre, w_up, w_down, norm_mlp_post):
    f32 = np.float32
    x = np.asarray(x, f32)
    B = x.shape[0]
    ncores = 8
    per = B // ncores
    if "nc" not in _CACHE:
        _CACHE["nc"] = build_nc()[0]
        _CACHE["consts"] = _consts()
    nc = _CACHE["nc"]
    consts = _CACHE["consts"]
    g_all = np.ascontiguousarray(np.stack([np.asarray(norm_mix_pre, f32)[0], np.asarray(norm_mix_post, f32)[0],
                                           np.asarray(norm_mlp_pre, f32)[0], np.asarray(norm_mlp_post, f32)[0]]))
    smallv = np.ascontiguousarray(np.concatenate([np.asarray(b_gate, f32)[0].reshape(16, 128).T,
                                                  np.asarray(pool_scale, f32)[0].reshape(4, 128).T], axis=1))
    shared = dict(
        w_in=np.ascontiguousarray(np.asarray(w_in, f32)[0]),
        w_bp=np.ascontiguousarray(np.asarray(w_branch_pool, f32)[0]),
        w_ba=np.ascontiguousarray(np.asarray(w_branch_attn, f32)[0]),
        w_out=np.ascontiguousarray(np.asarray(w_out, f32)[0]),
        w_up=np.ascontiguousarray(np.asarray(w_up, f32)[0]),
        w_down=np.ascontiguousarray(np.asarray(w_down, f32)[0]),
        pool_w=np.ascontiguousarray(np.asarray(pool_w, f32)[0].reshape(512, 128)),
        g_all=g_all, smallv=smallv, **consts)
    in_maps = []
    for c in range(ncores):
        m = dict(shared)
        m["x"] = np.ascontiguousarray(x[c * per:(c + 1) * per].reshape(per * S, D))
        in_maps.append(m)
    res = run_bass_kernel_spmd(nc, in_maps, core_ids=list(range(ncores)))
    outs = [np.asarray(r["out"], f32).reshape(per, S, D) for r in res.results]
    return np.concatenate(outs, axis=0)
```

```python
import contextlib
import numpy as np
import ml_dtypes
import concourse.bass as bass
import concourse.mybir as mybir
from concourse.bass_utils import run_bass_kernel_spmd

F32 = mybir.dt.float32
BF16 = mybir.dt.bfloat16
AF = mybir.ActivationFunctionType
ALU = mybir.AluOpType
AX = mybir.AxisListType

COMPUTE = ("pe", "act", "dve", "pool")

D = 1024
S = 2048
NSEQ = 2
H = 8
DH = 64
NB = 8
G = 512
NG = S // G
EPS = 1e-6
NEG = -30000.0
NSLOT = 4
SLOT = 8192


class V:
    __slots__ = ("ap", "key", "lo", "hi")

    def __init__(self, ap, key, lo, hi):
        self.ap, self.key, self.lo, self.hi = ap, key, lo, hi

    def re(self, pat, **kw):
        return V(self.ap.rearrange(pat, **kw), self.key, self.lo, self.hi)

    def __getitem__(self, idx):
        return V(self.ap[idx], self.key, self.lo, self.hi)

    def bc(self, shape):
        return V(self.ap.to_broadcast(shape), self.key, self.lo, self.hi)


class Buf:
    def __init__(self, nc, name, nbytes, psum=False):
        self.name = name
        self.nbytes = nbytes
        self.psum = psum
        if psum:
            self.t = nc.alloc_psum_tensor(name, [128, nbytes // 4], F32)
        else:
            self.t = nc.alloc_sbuf_tensor(name, [128, nbytes // 2], BF16)

    def v(self, lo, hi, dt=BF16):
        if self.psum:
            ap = self.t[:, lo // 4:hi // 4]
            if dt != F32:
                ap = ap.bitcast(dt)
        else:
            ap = self.t[:, lo // 2:hi // 2]
            if dt != BF16:
                ap = ap.bitcast(dt)
        return V(ap, self.name, lo, hi)


class Prog:
    def __init__(self, nc):
        self.nc = nc
        self.ops = {e: [] for e in COMPUTE + ("sp",)}
        self.count = {e: 0 for e in COMPUTE}
        self.seen = {e: {} for e in COMPUTE + ("sp",)}
        self.hist = {}
        self.known = {}
        self.dma_count = {}
        self.sem_names = set(COMPUTE)
        self.marks = []

    def mark(self, label):
        self.marks.append((label, dict(self.count)))

    def _deps(self, eng, reads, writes):
        deps = {}
        for r in reads:
            for (lo, hi, kind, tok) in self.hist.get(r.key, ()):
                if kind == "w" and lo < r.hi and r.lo < hi:
                    if deps.get(tok[0], 0) < tok[1]:
                        deps[tok[0]] = tok[1]
        for w in writes:
            for (lo, hi, kind, tok) in self.hist.get(w.key, ()):
                if lo < w.hi and w.lo < hi:
                    if deps.get(tok[0], 0) < tok[1]:
                        deps[tok[0]] = tok[1]
        waits = []
        seen = self.seen[eng]
        for sem, val in deps.items():
            if sem == eng and eng == "pe":
                continue
            if seen.get(sem, 0) >= val:
                continue
            waits.append((sem, val))
        for sem, val in waits:
            if seen.get(sem, 0) < val:
                seen[sem] = val
            kn = self.known.get((sem, val))
            if kn:
                for s2, v2 in kn.items():
                    if seen.get(s2, 0) < v2:
                        seen[s2] = v2
        return waits

    def _record(self, tok, reads, writes):
        for w in writes:
            lst = self.hist.setdefault(w.key, [])
            lst[:] = [rec for rec in lst if not (w.lo <= rec[0] and rec[1] <= w.hi)]
            lst.append((w.lo, w.hi, "w", tok))
        for r in reads:
            lst = self.hist.setdefault(r.key, [])
            lst[:] = [rec for rec in lst if not (rec[2] == "r" and rec[3][0] == tok[0]
                                                  and r.lo <= rec[0] and rec[1] <= r.hi)]
            lst.append((r.lo, r.hi, "r", tok))

    def op(self, eng, fn, reads=(), writes=()):
        reads = [r for r in reads if isinstance(r, V)]
        writes = [w for w in writes if isinstance(w, V)]
        waits = self._deps(eng, reads, writes)
        self.count[eng] += 1
        tok = (eng, self.count[eng])
        self.known[tok] = dict(self.seen[eng])
        self._record(tok, reads, writes)
        self.ops[eng].append((waits, fn, (eng, 1)))
        return tok

    def dma(self, sem, out, in_, queue="sp", extra_reads=()):
        self.sem_names.add(sem)
        reads = [in_] if isinstance(in_, V) else []
        writes = [out] if isinstance(out, V) else []
        waits = self._deps(queue, reads + list(extra_reads), writes)
        self.dma_count[sem] = self.dma_count.get(sem, 0) + 1
        tok = (sem, 16 * self.dma_count[sem])
        self.known[tok] = dict(self.seen[queue])
        self._record(tok, reads, writes)
        o = out.ap if isinstance(out, V) else out
        i = in_.ap if isinstance(in_, V) else in_
        self.ops[queue].append((waits, lambda e: e.dma_start(out=o, in_=i), (sem, 16)))
        return tok

    def final_wait(self, eng="sp"):
        waits = []
        for sem, cnt in self.dma_count.items():
            if self.seen[eng].get(sem, 0) < 16 * cnt:
                waits.append((sem, 16 * cnt))
        self.ops[eng].append((waits, None, None))

    def mm(self, out, lhsT, rhs, start=True, stop=True):
        return self.op("pe", lambda e: e.matmul(out.ap, lhsT=lhsT.ap, rhs=rhs.ap, start=start, stop=stop),
                       reads=[lhsT, rhs], writes=[out])

    def tr(self, out, in_, ident):
        return self.op("pe", lambda e: e.transpose(out=out.ap, in_=in_.ap, identity=ident.ap),
                       reads=[in_, ident], writes=[out])

    def act(self, out, in_, func, bias=None, scale=None, accum_out=None):
        kw = {}
        rd = [in_]
        wr = [out]
        if bias is not None:
            kw["bias"] = bias.ap if isinstance(bias, V) else bias
            rd.append(bias)
        if scale is not None:
            kw["scale"] = scale.ap if isinstance(scale, V) else scale
            rd.append(scale)
        if accum_out is not None:
            kw["accum_out"] = accum_out.ap
            wr.append(accum_out)
        return self.op("act", lambda e: e.activation(out=out.ap, in_=in_.ap, func=func, **kw), reads=rd, writes=wr)

    def copy(self, eng, out, in_):
        if eng == "act":
            return self.act(out, in_, AF.Copy)
        return self.op(eng, lambda e: e.tensor_copy(out=out.ap, in_=in_.ap), reads=[in_], writes=[out])

    def tt(self, eng, out, in0, in1, op):
        return self.op(eng, lambda e: e.tensor_tensor(out=out.ap, in0=in0.ap, in1=in1.ap, op=op),
                       reads=[in0, in1], writes=[out])

    def ts(self, eng, out, in0, s1, op0, s2=None, op1=None):
        a1 = s1.ap if isinstance(s1, V) else s1
        a2 = s2.ap if isinstance(s2, V) else s2
        kw = {}
        if op1 is not None:
            kw["op1"] = op1
        return self.op(eng, lambda e: e.tensor_scalar(out=out.ap, in0=in0.ap, scalar1=a1, scalar2=a2, op0=op0, **kw),
                       reads=[in0, s1, s2], writes=[out])

    def stt(self, eng, out, in0, scalar, in1, op0, op1):
        a = scalar.ap if isinstance(scalar, V) else scalar
        return self.op(eng, lambda e: e.scalar_tensor_tensor(out=out.ap, in0=in0.ap, scalar=a, in1=in1.ap,
                                                             op0=op0, op1=op1),
                       reads=[in0, scalar, in1], writes=[out])

    def reduce(self, eng, out, in_, op, axis=AX.X):
        return self.op(eng, lambda e: e.tensor_reduce(out=out.ap, in_=in_.ap, axis=axis, op=op),
                       reads=[in_], writes=[out])

    def recip(self, out, in_):
        return self.op("dve", lambda e: e.reciprocal(out=out.ap, in_=in_.ap), reads=[in_], writes=[out])

    def memset(self, eng, out, val):
        return self.op(eng, lambda e: e.memset(out.ap, val), writes=[out])

    def emit(self):
        nc = self.nc
        names = sorted(self.sem_names)
        sems = {}
        with contextlib.ExitStack() as st:
            for n in names:
                sems[n] = st.enter_context(nc.semaphore("s_" + n))
            block = st.enter_context(nc.Block())

            def run(eng_name):
                def body(e):
                    for waits, fn, inc in self.ops[eng_name]:
                        for sem, val in waits:
                            e.wait_ge(sems[sem], val)
                        if fn is not None:
                            fn(e).then_inc(sems[inc[0]], inc[1])
                return body

            block.sync(run("sp"))
            block.tensor(run("pe"))
            block.scalar(run("act"))
            block.vector(run("dve"))
            block.gpsimd(run("pool"))


def _consts():
    bf = ml_dtypes.bfloat16
    ident = np.eye(128, dtype=np.float32).astype(bf)
    tri = (np.arange(128)[None, :] >= np.arange(128)[:, None]).astype(np.float32).astype(bf)
    slopes = 2.0 ** (-8.0 * np.arange(1, H + 1) / H)
    t = np.arange(S)
    a, b = t // 256, t % 256
    kaug = np.zeros((32, H, S), np.float32)
    qaug = np.zeros((S, H, 96), np.float32)
    for h in range(H):
        sl = slopes[h]
        kaug[0, h] = 1.0
        kaug[1, h] = 1.0
        kaug[2, h] = sl * b
        kaug[3, h] = sl * 256.0 * a
        for n in range(NB):
            kaug[4 + n, h] = (a == n)
        qaug[:, h, 64] = -sl * b
        qaug[:, h, 65] = -sl * 256.0 * a
        qaug[:, h, 66] = 1.0
        qaug[:, h, 67] = 1.0
        for n in range(NB):
            allowed = (n == a) | ((n < a) & (a <= 3))
            qaug[:, h, 68 + n] = np.where(allowed, 0.0, NEG)
    qaug = qaug.reshape(S // 128, 128, H * 96)
    rcw = np.ones((128, 4, 16), np.float32)
    for grp in range(4):
        w_ = 2 ** (grp + 1)
        for tt_ in range(w_ - 1):
            rcw[:, grp, tt_] = w_ / (tt_ + 1.0)
    return dict(c_ident=ident, c_tri=tri, c_kaug=kaug.reshape(32, H * S).astype(bf),
                c_qaug=qaug.astype(bf), c_rcw=np.ascontiguousarray(rcw.reshape(128, 64)))


def build_nc():
    nc = bass.Bass("TRN2", target_bir_lowering=False)
    T = NSEQ * S

    def din(name, shape, dt=F32):
        return nc.dram_tensor(name, list(shape), dt, kind="ExternalInput").ap()

    x = din("x", [T, D])
    w_in = din("w_in", [D, 4096])
    w_bp = din("w_bp", [512, D])
    w_ba = din("w_ba", [512, D])
    w_out = din("w_out", [D, D])
    w_up = din("w_up", [D, 4096])
    w_down = din("w_down", [4096, D])
    pool_w = din("pool_w", [512, 128])
    g_all = din("g_all", [4, D])
    smallv = din("smallv", [128, 24])
    c_ident = din("c_ident", [128, 128], BF16)
    c_tri = din("c_tri", [128, 128], BF16)
    c_kaug = din("c_kaug", [32, H * S], BF16)
    c_qaug = din("c_qaug", [S // 128, 128, H * 96], BF16)
    c_rcw = din("c_rcw", [128, 64])
    out = nc.dram_tensor("out", [T, D], F32, kind="ExternalOutput").ap()

    def scr(name, shape):
        return nc.dram_tensor(name, list(shape), BF16, kind="Internal").ap()

    s_win = scr("s_win", [D, 2048])
    s_wg = scr("s_wg", [D, 2048])
    s_wmix = scr("s_wmix", [512, 2048])
    s_wout = scr("s_wout", [D, D])
    s_wup = scr("s_wup", [D, 4096])
    s_wdown = scr("s_wdown", [4096, D])
    s_pw = scr("s_pw", [512, 128])

    p = Prog(nc)

    b_ident = Buf(nc, "ident", 256)
    b_tri = Buf(nc, "tri", 256)
    b_g = Buf(nc, "gbc", 4 * 4096)
    b_small = Buf(nc, "small", 1024)
    b_pw = Buf(nc, "poolw", 1024)
    b_stats = Buf(nc, "stats", 512)
    b_ring = Buf(nc, "ring", NSLOT * SLOT)
    b_pmT = Buf(nc, "pmT", 16384)
    b_attnT = Buf(nc, "attnT", 16384)
    ARENA = 126976
    b_ar = Buf(nc, "arena", ARENA)
    PS = [Buf(nc, f"ps{i}", 2048, psum=True) for i in range(8)]

    def bank(i):
        return PS[i].v(0, 2048, F32)

    ident = b_ident.v(0, 256)
    tri = b_tri.v(0, 256)
    gbc = [b_g.v(i * 4096, (i + 1) * 4096, F32) for i in range(4)]
    bgate = b_small.v(0, 64, F32)
    pscale = b_small.v(64, 80, F32)
    winv = b_small.v(80, 96, F32)
    pscw = b_small.v(96, 112, F32)
    rcw = b_small.v(128, 384, F32).re("p (g t) -> p g t", g=4)
    poolw = b_pw.v(0, 1024).re("p (g c) -> p g c", g=4)
    pmT = b_pmT.v(0, 16384).re("p (g t) -> p g t", g=4)
    attnT = b_attnT.v(0, 16384).re("p (i t) -> p i t", i=4)

    QA, KA, VA, WA = 0, 32768, 65536, 90112
    q_aug = b_ar.v(QA, QA + 32768).re("p (h t) -> p h t", h=H)
    k_aug = b_ar.v(KA, KA + 32768).re("p (h t) -> p h t", h=H)
    v_aug = b_ar.v(VA, VA + 24576).re("p (k c) -> p k c", k=16)

    def wa(lo, hi, dt=BF16):
        return b_ar.v(WA + lo, WA + hi, dt)

    def ar(lo, hi, dt=BF16):
        return b_ar.v(lo, hi, dt)

    stat_i = [0]

    def stat():
        i = stat_i[0] % 16
        stat_i[0] += 1
        st = b_stats.v(i * 32, i * 32 + 32, F32)
        p.memset("dve", st[:, 0:2], 0.0)
        return st

    def rstd_from(st, ncols):
        src = st[:, 0:1]
        if ncols == 2:
            p.tt("dve", st[:, 2:3], st[:, 0:1], st[:, 1:2], ALU.add)
            src = st[:, 2:3]
        p.act(st[:, 4:5], src, AF.Sqrt, scale=1.0 / D, bias=EPS)
        p.recip(st[:, 5:6], st[:, 4:5])
        return st[:, 5:6]

    cn = [0]

    def cdma(o, i):
        cn[0] += 1
        p.dma(f"c{cn[0]}", o, i)

    cdma(ident, c_ident)
    cdma(tri, c_tri)
    for i in range(4):
        cdma(gbc[i], g_all[i].partition_broadcast(128))
    cdma(b_small.v(0, 96, F32), smallv)
    p.tt("dve", pscw, pscale, winv, ALU.mult)
    cdma(b_small.v(128, 384, F32), c_rcw)

    def scr_v(ap, key):
        return V(ap, key, 0, 1)

    cast_extra = []

    def cdcast(key, dst, src):
        p.dma("k_" + key, scr_v(dst, key), src, queue="pool", extra_reads=cast_extra)

    def cast(key, dst, src, rows):
        n = dst.shape[0]
        for r0 in range(0, n, rows):
            cdcast(key, dst[r0:r0 + rows], src[r0:r0 + rows])

    cast("win", s_win, w_in[:, 0:2048], 256)
    cast("pw", s_pw, pool_w, 512)
    bg_casts = []
    for hh in range(2):
        bg_casts.append(("wmix", s_wmix[:, 1024 * hh:1024 * hh + 512], w_bp[:, 512 * hh:512 * hh + 512]))
        bg_casts.append(("wmix", s_wmix[:, 1024 * hh + 512:1024 * hh + 1024], w_ba[:, 512 * hh:512 * hh + 512]))
    for r in range(4):
        bg_casts.append(("wg", s_wg[:, 512 * r:512 * r + 256], w_in[:, 2048 + 256 * r:2048 + 256 * r + 256]))
        bg_casts.append(("wg", s_wg[:, 512 * r + 256:512 * r + 512], w_in[:, 3072 + 256 * r:3072 + 256 * r + 256]))
    for r0 in range(0, D, 256):
        bg_casts.append(("wout", s_wout[r0:r0 + 256], w_out[r0:r0 + 256]))
    for r0 in range(0, D, 256):
        bg_casts.append(("wup", s_wup[r0:r0 + 256], w_up[r0:r0 + 256]))
    for r0 in range(0, 4096, 512):
        bg_casts.append(("wdown", s_wdown[r0:r0 + 512], w_down[r0:r0 + 512]))

    def bg_cast(n=1):
        for _ in range(n):
            if bg_casts:
                key, dst, src = bg_casts.pop(0)
                cdcast(key, dst, src)

    cdma(b_pw.v(0, 1024), scr_v(s_pw.rearrange("(g p) c -> p g c", p=128), "pw"))

    def piece_src(kind, idx):
        if kind == "win":
            return scr_v(s_win.rearrange("(kc p) n -> p kc n", p=128)[:, :, 512 * idx:512 * idx + 512], "win"), 8
        if kind == "wg":
            return scr_v(s_wg.rearrange("(kc p) n -> p kc n", p=128)[:, :, 512 * idx:512 * idx + 512], "wg"), 8
        if kind == "wmix":
            return scr_v(s_wmix.rearrange("(kc p) n -> p kc n", p=128)[:, :, 1024 * idx:1024 * idx + 1024], "wmix"), 4
        if kind == "wout":
            return scr_v(s_wout.rearrange("(kc p) n -> p kc n", p=128)[:, :, 512 * idx:512 * idx + 512], "wout"), 8
        if kind == "wup":
            return scr_v(s_wup.rearrange("(kc p) n -> p kc n", p=128)[:, :, 512 * idx:512 * idx + 512], "wup"), 8
        if kind == "wdown":
            half, kg = idx // 4, idx % 4
            return scr_v(s_wdown.rearrange("(kc p) n -> p kc n", p=128)[:, 8 * kg:8 * kg + 8, 512 * half:512 * half + 512],
                         "wdown"), 8
        raise KeyError(kind)

    MIXP = [("wmix", 0), ("wg", 0), ("wg", 1), ("wmix", 1), ("wg", 2), ("wg", 3)]
    pieces = []
    for s in range(NSEQ):
        for g in range(NG):
            pieces += [("A", s, g, "win", i) for i in range(4)]
        pieces += [("C", s, 0) + m for m in MIXP]
        for g in range(NG):
            pieces += [("C", s, g, "wout", i) for i in range(2)]
            if g + 1 < NG:
                pieces += [("C", s, g + 1) + m for m in MIXP]
            pieces += [("C", s, g, "wup", i) for i in range(8)]
            pieces += [("C", s, g, "wdown", i) for i in range(8)]
    piece_idx = {pc: i for i, pc in enumerate(pieces)}
    ring = dict(next=0, free=list(range(NSLOT)), loaded={})

    def ring_fill():
        while ring["free"] and ring["next"] < len(pieces):
            slot = ring["free"].pop(0)
            i = ring["next"]
            ring["next"] += 1
            src, kc = piece_src(pieces[i][3], pieces[i][4])
            dst = b_ring.v(slot * SLOT, (slot + 1) * SLOT).re("p (kc n) -> p kc n", kc=kc)
            p.dma(f"ring{slot}", dst, src)
            ring["loaded"][i] = (slot, dst)

    def ring_get(pc):
        i = piece_idx[pc]
        ring_fill()
        assert i in ring["loaded"], ("ring piece not loaded (deadlock in schedule)", pc, ring)
        return ring["loaded"][i][1]

    def ring_rel(pc):
        i = piece_idx[pc]
        slot, _ = ring["loaded"].pop(i)
        ring["free"].append(slot)
        ring_fill()

    evac_rr = [0]

    def evac_eng():
        evac_rr[0] += 1
        return "act" if evac_rr[0] % 2 else "dve"

    fctr = [0]

    def fb():
        b = PS[4 + fctr[0] % 4]
        fctr[0] += 1
        return b

    def transpose_tile(xn_v, dstT, tt, pbuf):
        pst = pbuf.v(0, 2048, BF16)
        for kc in range(8):
            p.tr(pst[:, kc * 128:(kc + 1) * 128], xn_v[:, kc * 128:(kc + 1) * 128], ident)
        p.copy(evac_eng(), dstT[:, :, tt * 128:(tt + 1) * 128], pst.re("p (kc t) -> p kc t", kc=8))

    def norm_in(xv, xnv, gain):
        st = stat()
        p.act(xnv, xv, AF.Square, accum_out=st[:, 0:1])
        r = rstd_from(st, 1)
        p.stt("dve", xnv, xv, r, gain, ALU.mult, ALU.mult)

    import os as _os
    _stop = _os.environ.get("KSTOP", "")

    class _Stop(Exception):
        pass

    def stop_at(tag):
        if _stop and _stop == tag:
            raise _Stop()

    try:
      for s in range(NSEQ):
          tok0 = s * S
          p.mark(f"s{s}.A")
          cdma(k_aug[64:96], c_kaug.rearrange("p (h t) -> p h t", h=H))
          ones_v = v_aug.re("p k (i b c) -> p k i b c", i=4, b=3)[:, :, :, 1, :]
          p.memset("pool", ones_v, 1.0)

          xa = [wa(0, 4096, F32), wa(4096, 8192, F32)]
          xn = [wa(8192 + i * 2048, 8192 + (i + 1) * 2048) for i in range(4)]
          P4 = [wa(16384 + i * 2112, 16384 + (i + 1) * 2112, F32) for i in range(4)]
          T0 = wa(24832, 26944, F32)
          T1 = wa(26944, 29056, F32)
          yb = [wa(29056 + i * 1024, 29056 + (i + 1) * 1024) for i in range(4)]
          Pw = wa(33152, 35264, F32)
          uTa = [b_attnT.v(i * 8192, (i + 1) * 8192).re("p (kc t) -> p kc t", kc=8) for i in range(2)]
          actr = [0]

          def a_ld_norm(g):
              for tt in range(4):
                  k = actr[0] % 2
                  actr[0] += 1
                  r0 = tok0 + g * G + tt * 128
                  p.dma(f"xa{k}", xa[k], x[r0:r0 + 128, :])
                  norm_in(xa[k], xn[tt], gbc[0])

          def a_tr(g):
              for tt in range(4):
                  transpose_tile(xn[tt], uTa[g % 2], tt, PS[6])

          a_ld_norm(0)
          a_tr(0)
          for g in range(NG):
              uT = uTa[g % 2]
              cols = slice(g * G, (g + 1) * G)
              for grp in range(4):
                  if g == 0:
                      p.memset("pool", P4[grp][:, 0:16], 0.0)
                  else:
                      p.copy("pool", P4[grp][:, 0:16], P4[grp][:, 512:528])
              if g + 1 < NG:
                  a_ld_norm(g + 1)

              def proj(kind, sub):
                  pc = ("A", s, g, "win", kind)
                  w = ring_get(pc)
                  bk = bank((kind * 4 + sub) % 4)
                  for kc in range(8):
                      p.mm(bk, w[:, kc, sub * 128:(sub + 1) * 128], uT[:, kc, :], start=(kc == 0), stop=(kc == 7))
                  if kind == 0:
                      p.copy("act", P4[sub][:, 16:528], bk)
                  elif kind == 1:
                      p.act(q_aug[0:64, 2 * sub, cols], bk[0:64], AF.Copy, scale=0.125)
                      p.act(q_aug[0:64, 2 * sub + 1, cols], bk[64:128], AF.Copy, scale=0.125)
                  else:
                      p.copy("act", k_aug[0:64, 2 * sub, cols], bk[0:64])
                      p.copy("act", k_aug[0:64, 2 * sub + 1, cols], bk[64:128])
                  if sub == 3:
                      ring_rel(pc)

              for sub in range(4):
                  proj(0, sub)
              for grp in range(4):
                  Pg = P4[grp]
                  w_ = 2 ** (grp + 1)
                  p.tt("pool", T0[:, 1:528], Pg[:, 1:528], Pg[:, 0:527], ALU.add)
                  last = T0
                  if grp >= 1:
                      p.tt("pool", T1[:, 3:528], T0[:, 3:528], T0[:, 1:526], ALU.add)
                      last = T1
                  if grp >= 2:
                      p.tt("pool", T0[:, 7:528], T1[:, 7:528], T1[:, 3:524], ALU.add)
                      last = T0
                  if grp >= 3:
                      p.tt("pool", T1[:, 15:528], T0[:, 15:528], T0[:, 7:520], ALU.add)
                      last = T1
                  if g == 0:
                      n = w_ - 1
                      p.tt("pool", last[:, 16:16 + n], last[:, 16:16 + n], rcw[:, grp, 0:n], ALU.mult)
                  p.stt("dve", yb[grp], Pg[:, 16:528], -float(w_), last[:, 16:528], ALU.mult, ALU.add)
              if s == 0:
                  bg_cast(4)
              for kind in (1, 2):
                  for sub in range(4):
                      proj(kind, sub)
              if g + 1 < NG:
                  a_tr(g + 1)
              pc = ("A", s, g, "win", 3)
              w = ring_get(pc)
              for tt in range(4):
                  bk = bank(4 + tt % 2)
                  for kc in range(8):
                      p.mm(bk, uT[:, kc, tt * 128:(tt + 1) * 128], w[:, kc, :], start=(kc == 0), stop=(kc == 7))
                  kt = g * 4 + tt
                  dst = v_aug[:, kt, :].re("p (i b c) -> p i b c", i=4, b=3)
                  src = bk.re("p (i b c) -> p i b c", i=4, b=2)
                  p.copy("act", dst[:, :, 0, :], src[:, :, 0, :])
                  p.copy("dve", dst[:, :, 2, :], src[:, :, 1, :])
              ring_rel(pc)
              for grp in range(4):
                  pb = bank(4 + grp)
                  p.mm(pb, poolw[:, grp, :], yb[grp])
                  p.act(pmT[:, grp, cols], pb, AF.Copy, scale=pscw[:, grp:grp + 1])

          stop_at(f"A{s}")
          p.mark(f"s{s}.B")
          pt = [wa(i * 1024, (i + 1) * 1024) for i in range(4)]
          rden = [wa(4096 + i * 2048, 4096 + (i + 1) * 2048, F32) for i in range(2)]
          mbq = [wa(8192 + i * 1536, 8192 + (i + 1) * 1536).re("p (h c) -> p h c", h=H) for i in range(4)]
          g_sb = wa(14336, 14592, F32).re("p (h n) -> p h n", h=H)
          kms = wa(16384, 16640, F32)
          kmT = wa(16640, 16768)

          p.reduce("dve", kms[0:64], k_aug[0:64].re("p h (n j) -> p (h n) j", n=NB), ALU.add)
          p.ts("dve", kmT[0:64], kms[0:64], 1.0 / 256, ALU.mult)

          def mask_pre(j):
              for qt in range(4 * j, 4 * j + 4):
                  qb = qt // 2
                  mb = mbq[qt % 4]
                  p.dma(f"mb{qt % 4}", mb, c_qaug[qt].rearrange("p (h c) -> p h c", h=H))
                  if qb >= 4:
                      npast = qb
                      psG = PS[6].v(0, 256, F32)
                      for h in range(H):
                          p.mm(psG[:, h * 8:(h + 1) * 8], q_aug[0:64, h, qt * 128:(qt + 1) * 128], kmT[0:64, h * 8:(h + 1) * 8])
                      p.copy("dve", g_sb, psG.re("p (h n) -> p h n", h=H))
                      gt = g_sb[:, :, 0:npast]
                      cmpb = wa(14592, 14592 + H * npast * npast * 4, F32).re("p (h n m) -> p h n m", h=H, n=npast)
                      in0 = V(gt.ap.unsqueeze(2).to_broadcast([128, H, npast, npast]), gt.key, gt.lo, gt.hi)
                      in1 = V(gt.ap.unsqueeze(3).to_broadcast([128, H, npast, npast]), gt.key, gt.lo, gt.hi)
                      p.tt("dve", cmpb, in0, in1, ALU.is_gt)
                      rank = wa(16160, 16160 + H * npast * 4, F32).re("p (h n) -> p h n", h=H)
                      p.reduce("dve", rank, cmpb, ALU.add)
                      p.ts("dve", mb[:, :, 68:68 + npast], rank, 2.5, ALU.is_gt, NEG, ALU.mult)

          def mask_post(j):
              for qt in range(4 * j, 4 * j + 4):
                  mb = mbq[qt % 4]
                  psT = PS[7 - qt % 2].v(0, 2048, BF16)
                  for h in range(H):
                      p.tr(psT[0:96, h * 128:(h + 1) * 128], mb[:, h, :], ident)
                  p.copy("act", q_aug[64:96, :, qt * 128:(qt + 1) * 128], psT[64:96].re("p (h q) -> p h q", h=H))

          tiles = []
          first_of_j = {}
          for j in range(4):
              first_of_j[len(tiles)] = j
              for i in range(4):
                  for e in range(2):
                      h = 2 * i + e
                      nk = 4 * j + 4
                      for kt in range(nk):
                          c0 = 0 if kt < 4 * j else 128 * (kt - 4 * j)
                          tiles.append((i, j, e, h, kt, c0, nk))
          LOOK = 3
          acc_ctr = [0]
          tri_ctr = [0]

          def emit_score(n):
              i, j, e, h, kt, c0, nk = tiles[n]
              sb = bank(n % 4)
              p.mm(sb[:, c0:512], k_aug[0:96, h, kt * 128:(kt + 1) * 128], q_aug[0:96, h, j * 512 + c0:(j + 1) * 512])
              ptv = pt[n % 4]
              p.act(ptv[:, c0:512], sb[:, c0:512], AF.Exp)
              if kt >= 4 * j:
                  p.tt("pool", ptv[:, c0:c0 + 128], ptv[:, c0:c0 + 128], tri, ALU.mult)
                  tri_ctr[0] += 1
                  if s == 0 and tri_ctr[0] % 6 == 0:
                      bg_cast(1)

          def emit_pv(n):
              i, j, e, h, kt, c0, nk = tiles[n]
              if kt == 0:
                  acc_ctr[0] += 1
              ob = bank(4 + acc_ctr[0] % 2)
              lo = 192 * i + 64 * e
              p.mm(ob[:, c0:512], v_aug[:, kt, lo:lo + 128], pt[n % 4][:, c0:512], start=(kt == 0), stop=(kt == nk - 1))
              if kt == nk - 1:
                  cols = slice(j * 512, (j + 1) * 512)
                  rd = rden[acc_ctr[0] % 2]
                  if e == 0:
                      p.recip(rd[0:64], ob[64:128])
                      p.tt("dve", attnT[0:64, i, cols], ob[0:64], rd[0:64], ALU.mult)
                  else:
                      p.recip(rd[64:128], ob[0:64])
                      p.tt("dve", attnT[64:128, i, cols], ob[64:128], rd[64:128], ALU.mult)

          mask_pre(0)
          for n in range(len(tiles) + LOOK):
              if n in first_of_j:
                  j = first_of_j[n]
                  mask_post(j)
                  if j + 1 < 4:
                      mask_pre(j + 1)
              if n < len(tiles):
                  emit_score(n)
              if n >= LOOK:
                  emit_pv(n - LOOK)

          stop_at(f"B{s}")
          bg_cast(len(bg_casts))
          xh = [ar(i * 4096, (i + 1) * 4096, F32) for i in range(4)]
          xtmp = [ar(16384 + i * 4096, 16384 + (i + 1) * 4096, F32) for i in range(2)]
          xnl = [ar(24576 + i * 2048, 24576 + (i + 1) * 2048) for i in range(4)]
          xnc = [ar(32768 + i * 2048, 32768 + (i + 1) * 2048) for i in range(4)]
          uT2 = ar(40960, 49152).re("p (kc t) -> p kc t", kc=8)
          sg = [ar(49152 + i * 2048, 49152 + (i + 1) * 2048, F32) for i in range(4)]
          t12 = [ar(57344 + i * 2048, 57344 + (i + 1) * 2048, F32) for i in range(4)]
          dsb = t12
          mT = ar(65536, 73728).re("p (kc t) -> p kc t", kc=8)
          u2T = ar(73728, 81920).re("p (kc t) -> p kc t", kc=8)
          aT = ar(81920, 114688).re("p (f t) -> p f t", f=32)
          tmpn = [ar(114688 + i * 4096, 114688 + (i + 1) * 4096, F32) for i in range(2)]
          rl = [ar(122880 + i * 2048, 122880 + (i + 1) * 2048, F32) for i in range(2)]
          rlj = [ar(122880 + i * 2048, 122880 + (i + 1) * 2048) for i in range(2)]
          lctr = [0]

          def rows(g, tt):
              r0 = tok0 + g * G + tt * 128
              return slice(r0, r0 + 128)

          def c_x_reload(g):
              for tt in range(4):
                  p.dma(f"xh{tt}", xh[tt], x[rows(g, tt), :])

          def c_ld_norm(g):
              for tt in range(4):
                  k = lctr[0] % 2
                  lctr[0] += 1
                  p.dma(f"xl{k}", xtmp[k], x[rows(g, tt), :])
                  norm_in(xtmp[k], xnl[tt], gbc[0])

          def c_ld_tr(g):
              p.mark(f"s{s}.C{g}.ldtr")
              for tt in range(4):
                  transpose_tile(xnl[tt], uT2, tt, fb())

          def c_mix(g, hooks=None):
              p.mark(f"s{s}.C{g}.mix")
              cols = slice(g * G, (g + 1) * G)
              for c in range(8):
                  hh = c // 4
                  pmix = ("C", s, g, "wmix", hh)
                  wmix = ring_get(pmix)
                  pg_ = ("C", s, g, "wg", c // 2)
                  wg = ring_get(pg_)
                  bgp, bga, byp, bya = (fb().v(0, 2048, F32) for _ in range(4))
                  cc = (c % 4) * 128
                  gc = (c % 2) * 128
                  for kc in range(8):
                      p.mm(bgp, wg[:, kc, gc:gc + 128], uT2[:, kc, :], start=(kc == 0), stop=(kc == 7))
                  for kc in range(8):
                      p.mm(bga, wg[:, kc, 256 + gc:256 + gc + 128], uT2[:, kc, :], start=(kc == 0), stop=(kc == 7))
                  for kc in range(4):
                      p.mm(byp, wmix[:, kc, cc:cc + 128], pmT[:, kc, cols], start=(kc == 0), stop=(kc == 3))
                  for kc in range(4):
                      p.mm(bya, wmix[:, kc, 512 + cc:512 + cc + 128], attnT[:, kc, cols], start=(kc == 0), stop=(kc == 3))
                  s0, s1 = sg[(c % 2) * 2], sg[(c % 2) * 2 + 1]
                  t0, t1 = t12[(c % 2) * 2], t12[(c % 2) * 2 + 1]
                  p.act(s0, bgp, AF.Sigmoid, bias=bgate[:, c:c + 1])
                  p.act(s1, bga, AF.Sigmoid, bias=bgate[:, 8 + c:9 + c])
                  p.tt("dve", t0, byp, s0, ALU.mult)
                  p.tt("dve", t1, bya, s1, ALU.mult)
                  p.tt("pool", mT[:, c, :], t0, t1, ALU.add)
                  if c % 2 == 1:
                      ring_rel(pg_)
                  if c % 4 == 3:
                      ring_rel(pmix)
                  if hooks and c in hooks:
                      hooks[c]()

          def c_mo_mix(g):
              p.mark(f"s{s}.C{g}.mo")
              pc0, pc1 = ("C", s, g, "wout", 0), ("C", s, g, "wout", 1)
              w0 = ring_get(pc0)
              w1 = ring_get(pc1)
              sts = {}

              def partA(tt, b0, b1):
                  st = stat()
                  sts[tt] = st
                  xc = xnc[tt]
                  p.act(xc[:, 0:512], b0, AF.Square, accum_out=st[:, 0:1])
                  p.act(xc[:, 512:1024], b1, AF.Square, accum_out=st[:, 1:2])
                  tv = tmpn[tt % 2]
                  p.op("dve", lambda e, o=tv[:, 0:512], a=b0, b=gbc[1][:, 0:512]: e.tensor_tensor(out=o.ap, in0=a.ap, in1=b.ap, op=ALU.mult),
                       reads=[b0, gbc[1], xc], writes=[tv])
                  p.op("dve", lambda e, o=tv[:, 512:1024], a=b1, b=gbc[1][:, 512:1024]: e.tensor_tensor(out=o.ap, in0=a.ap, in1=b.ap, op=ALU.mult),
                       reads=[b1, gbc[1], xc], writes=[tv])

              def partB1(tt):
                  r = rstd_from(sts[tt], 2)
                  p.stt("dve", xh[tt], tmpn[tt % 2], r, xh[tt], ALU.mult, ALU.add)

              def partB2(tt):
                  norm_in(xh[tt], xnc[tt], gbc[2])

              for tt in range(4):
                  b0 = bank(2 * (tt % 2))
                  b1 = bank(2 * (tt % 2) + 1)
                  for bk, w in ((b0, w0), (b1, w1)):
                      for kc in range(8):
                          p.mm(bk, mT[:, kc, tt * 128:(tt + 1) * 128], w[:, kc, :], start=(kc == 0), stop=(kc == 7))
                  partA(tt, b0, b1)
                  if 1 <= tt <= 2:
                      partB1(tt - 1)
              ring_rel(pc0)
              ring_rel(pc1)
              if g + 1 < NG:
                  c_mix(g + 1, hooks={0: lambda: partB1(2), 1: lambda: partB1(3), 2: lambda: partB2(0),
                                      3: lambda: partB2(1), 4: lambda: partB2(2), 5: lambda: partB2(3)})
              else:
                  partB1(2)
                  partB1(3)
                  for tt in range(4):
                      partB2(tt)

          def c_u2tr(g):
              for tt in range(4):
                  transpose_tile(xnc[tt], u2T, tt, fb())

          def c_up(g):
              p.mark(f"s{s}.C{g}.up")
              for f in range(32):
                  pc = ("C", s, g, "wup", f // 4)
                  w = ring_get(pc)
                  bk = fb().v(0, 2048, F32)
                  fc = (f % 4) * 128
                  for kc in range(8):
                      p.mm(bk, w[:, kc, fc:fc + 128], u2T[:, kc, :], start=(kc == 0), stop=(kc == 7))
                  rv = rl[f % 2]
                  p.act(rv, bk, AF.Relu)
                  p.tt("pool" if f % 2 else "dve", aT[:, f, :], rv, rv, ALU.mult)
                  if f % 4 == 3:
                      ring_rel(pc)

          def c_down(g):
              p.mark(f"s{s}.C{g}.down")
              for half in range(2):
                  for kg in range(4):
                      pc = ("C", s, g, "wdown", half * 4 + kg)
                      w = ring_get(pc)
                      order = ([(k8, tt) for k8 in range(8) for tt in range(4)] if kg < 3
                               else [(k8, tt) for tt in range(4) for k8 in range(8)])
                      for k8, tt in order:
                          f = kg * 8 + k8
                          p.mm(bank(tt), aT[:, f, tt * 128:(tt + 1) * 128], w[:, k8, :], start=(f == 0), stop=(f == 31))
                      ring_rel(pc)
                  if half == 0:
                      for tt in range(4):
                          p.copy(evac_eng(), dsb[tt], bank(tt))
              sts = {}

              def partA(tt):
                  st = stat()
                  sts[tt] = st
                  jv = rlj[tt % 2]
                  p.act(jv[:, 0:512], dsb[tt], AF.Square, accum_out=st[:, 0:1])
                  p.act(jv[:, 512:1024], bank(tt), AF.Square, accum_out=st[:, 1:2])
                  tv = tmpn[tt % 2]
                  p.tt("dve", tv[:, 0:512], dsb[tt], gbc[3][:, 0:512], ALU.mult)
                  p.op("dve", lambda e, o=tv[:, 512:1024], a=bank(tt), b=gbc[3][:, 512:1024]: e.tensor_tensor(out=o.ap, in0=a.ap, in1=b.ap, op=ALU.mult),
                       reads=[bank(tt), gbc[3], jv], writes=[tv])

              def partB(tt):
                  r = rstd_from(sts[tt], 2)
                  p.stt("dve", xh[tt], tmpn[tt % 2], r, xh[tt], ALU.mult, ALU.add)
                  p.dma(f"st{tt}", out[rows(g, tt), :], xh[tt])

              for tt in range(4):
                  partA(tt)
                  if tt >= 1:
                      partB(tt - 1)
              partB(3)

          p.mark(f"s{s}.C")
          c_x_reload(0)
          c_ld_norm(0)
          c_ld_tr(0)
          c_mix(0)
          if NG > 1:
              c_ld_norm(1)
          for g in range(NG):
              if g + 1 < NG:
                  c_ld_tr(g + 1)
              c_mo_mix(g)
              c_u2tr(g)
              c_up(g)
              if g + 2 < NG:
                  c_ld_norm(g + 2)
              c_down(g)
              stop_at(f"Cdown{s}{g}")
              if g + 1 < NG:
                  c_x_reload(g + 1)
          stop_at(f"C{s}")
    except _Stop:
        pass
    p.mark("end")
    p.final_wait("sp")
    p.emit()
    return nc, p


_CACHE = {}


def kernel(x, norm_mix_pre, w_in, b_gate, pool_w, pool_scale, w_branch_pool, w_branch_attn, w_out,
           norm_mix_post, norm_mlp_pre, w_up, w_down, norm_mlp_post):
    f32 = np.float32
    x = np.asarray(x, f32)
    B = x.shape[0]
    ncores = 8
    per = B // ncores
    if "nc" not in _CACHE:
        _CACHE["nc"] = build_nc()[0]
        _CACHE["consts"] = _consts()
    nc = _CACHE["nc"]
    consts = _CACHE["consts"]
    g_all = np.ascontiguousarray(np.stack([np.asarray(norm_mix_pre, f32)[0], np.asarray(norm_mix_post, f32)[0],
                                           np.asarray(norm_mlp_pre, f32)[0], np.asarray(norm_mlp_post, f32)[0]]))
    winv_c = np.broadcast_to(np.array([1.0 / 2, 1.0 / 4, 1.0 / 8, 1.0 / 16], f32), (128, 4))
    smallv = np.ascontiguousarray(np.concatenate([np.asarray(b_gate, f32)[0].reshape(16, 128).T,
                                                  np.asarray(pool_scale, f32)[0].reshape(4, 128).T, winv_c], axis=1))
    shared = dict(
        w_in=np.ascontiguousarray(np.asarray(w_in, f32)[0]),
        w_bp=np.ascontiguousarray(np.asarray(w_branch_pool, f32)[0]),
        w_ba=np.ascontiguousarray(np.asarray(w_branch_attn, f32)[0]),
        w_out=np.ascontiguousarray(np.asarray(w_out, f32)[0]),
        w_up=np.ascontiguousarray(np.asarray(w_up, f32)[0]),
        w_down=np.ascontiguousarray(np.asarray(w_down, f32)[0]),
        pool_w=np.ascontiguousarray(np.asarray(pool_w, f32)[0].reshape(512, 128)),
        g_all=g_all, smallv=smallv, **consts)
    in_maps = []
    for c in range(ncores):
        m = dict(shared)
        m["x"] = np.ascontiguousarray(x[c * per:(c + 1) * per].reshape(per * S, D))
        in_maps.append(m)
    res = run_bass_kernel_spmd(nc, in_maps, core_ids=list(range(ncores)))
    outs = [np.asarray(r["out"], f32).reshape(per, S, D) for r in res.results]
    return np.concatenate(outs, axis=0)
```
